# Optimizing a Trainium2 kernel written in Bass

```python
import jax, jax.numpy as jnp
from jax import lax
import numpy as np

D_MODEL = 1024
BATCH = 1
SEQ = 16384
DEPTH = 1

CHUNK = 64
D_MIX = D_MODEL
CONV_WIDTH = D_MIX // 2
CONV_GROUPS = 8
CONV_K = 3
SGU_WIDTH = D_MIX - CONV_WIDTH
SGU_GROUPS = 4
SGU_BLOCK = 128
D_IN = 3 * CONV_WIDTH + 2 * SGU_WIDTH
PEER_HEADS = 8
N_KEYS = 128
N_EXPERTS = N_KEYS * N_KEYS
PEER_TOPK = 16
D_QUERY = 256
D_HALF = D_QUERY // 2
PEER_BLOCK = 128
EPS = 1e-6

kernel_name = "hybrid_shortconv_sgu_peer_block"


def rmsnorm(x, g):
    xf = x.astype(jnp.float32)
    xf = xf * lax.rsqrt(jnp.mean(xf * xf, axis=-1, keepdims=True) + EPS)
    return (xf * g.astype(jnp.float32)).astype(x.dtype)


def group_rmsnorm(y, g, groups):
    shp = y.shape
    yf = y.astype(jnp.float32).reshape(shp[:-1] + (groups, shp[-1] // groups))
    yf = yf * lax.rsqrt(jnp.mean(yf * yf, axis=-1, keepdims=True) + EPS)
    return (yf.reshape(shp) * g.astype(jnp.float32)).astype(y.dtype)


def short_conv_mixer(b_gate, c_gate, h, conv_w):
    z = c_gate * h
    seq = z.shape[1]
    zp = jnp.pad(z, ((0, 0), (CONV_K - 1, 0), (0, 0)))
    conv = sum(conv_w[k] * zp[:, k:k + seq] for k in range(CONV_K))
    return b_gate * conv


def spatial_gating_mixer(u, v, w_s, b_s, g_v):
    bsz, seq, _ = u.shape
    nb = seq // SGU_BLOCK
    v = group_rmsnorm(v, g_v, SGU_GROUPS)
    vb = v.reshape(bsz, nb, SGU_BLOCK, SGU_GROUPS, SGU_WIDTH // SGU_GROUPS)
    chunk_id = jnp.arange(SGU_BLOCK) // CHUNK
    mask = chunk_id[:, None] >= chunk_id[None, :]
    w = jnp.where(mask[None], w_s, jnp.zeros((), w_s.dtype))
    mixed = jnp.einsum('gij,bnjgc->bnigc', w, vb) + b_s.T[None, None, :, :, None]
    return u * mixed.reshape(bsz, seq, SGU_WIDTH)


def peer_ffn(x, w_q, sub_keys, expert_u, expert_v):
    bsz, seq, d = x.shape
    xt = x.reshape(-1, PEER_BLOCK, d)

    def block(xb):
        t = xb.shape[0]
        q = (xb @ w_q).reshape(t, PEER_HEADS, 2, D_HALF)
        s = jnp.einsum('thpd,hpkd->thpk', q, sub_keys).astype(jnp.float32)
        v1, i1 = lax.top_k(s[:, :, 0], PEER_TOPK)
        v2, i2 = lax.top_k(s[:, :, 1], PEER_TOPK)
        cand = (v1[..., :, None] + v2[..., None, :]).reshape(t, PEER_HEADS, PEER_TOPK * PEER_TOPK)
        cidx = (i1[..., :, None] * N_KEYS + i2[..., None, :]).reshape(t, PEER_HEADS, PEER_TOPK * PEER_TOPK)
        top, pos = lax.top_k(cand, PEER_TOPK)
        idx = jnp.take_along_axis(cidx, pos, axis=-1)
        gates = jax.nn.softmax(top, axis=-1)
        u_sel = expert_u[idx]
        act = jax.nn.gelu(jnp.einsum('thkd,td->thk', u_sel, xb).astype(jnp.float32), approximate=False)
        v_sel = expert_v[idx]
        return jnp.einsum('thk,thkd->td', (gates * act).astype(xb.dtype), v_sel)

    y = lax.map(block, xt)
    return y.reshape(bsz, seq, d)


def setup_inputs(seed: int = 0) -> dict:
    key = jax.random.key(seed)
    ks = jax.random.split(key, 16)
    f32 = jnp.float32
    L = DEPTH
    nrm = lambda k, shp, s: (jax.random.normal(k, shp, f32) * s).astype(f32)
    return {
        "x": nrm(ks[0], (BATCH, SEQ, D_MODEL), 1.0),
        "attn_norm_g": 1.0 + nrm(ks[1], (L, D_MODEL), 0.02),
        "w_in": nrm(ks[2], (L, D_MODEL, D_IN), D_MODEL ** -0.5),
        "conv_w": nrm(ks[3], (L, CONV_K, CONV_WIDTH), CONV_K ** -0.5),
        "sgu_w": nrm(ks[4], (L, SGU_GROUPS, SGU_BLOCK, SGU_BLOCK), SGU_BLOCK ** -0.5),
        "sgu_b": 1.0 + nrm(ks[5], (L, SGU_GROUPS, SGU_BLOCK), 0.02),
        "sgu_norm_g": 1.0 + nrm(ks[6], (L, SGU_WIDTH), 0.02),
        "out_norm_conv_g": 1.0 + nrm(ks[7], (L, CONV_WIDTH), 0.02),
        "out_norm_sgu_g": 1.0 + nrm(ks[8], (L, SGU_WIDTH), 0.02),
        "w_out": nrm(ks[9], (L, D_MIX, D_MODEL), D_MIX ** -0.5),
        "ffn_norm_g": 1.0 + nrm(ks[10], (L, D_MODEL), 0.02),
        "peer_w_q": nrm(ks[11], (L, D_MODEL, PEER_HEADS * D_QUERY), D_MODEL ** -0.5),
        "peer_sub_keys": nrm(ks[12], (L, PEER_HEADS, 2, N_KEYS, D_HALF), D_HALF ** -0.5),
        "peer_u": nrm(ks[13], (L, N_EXPERTS, D_MODEL), D_MODEL ** -0.5),
        "peer_v": nrm(ks[14], (L, N_EXPERTS, D_MODEL), PEER_HEADS ** -0.5),
        "final_norm_g": 1.0 + nrm(ks[15], (D_MODEL,), 0.02),
    }


def reference(x, attn_norm_g, w_in, conv_w, sgu_w, sgu_b, sgu_norm_g, out_norm_conv_g,
              out_norm_sgu_g, w_out, ffn_norm_g, peer_w_q, peer_sub_keys, peer_u, peer_v,
              final_norm_g):
    h = x
    for l in range(DEPTH):
        xn = rmsnorm(h, attn_norm_g[l])
        proj = xn @ w_in[l]
        c0, c1, c2, c3 = CONV_WIDTH, 2 * CONV_WIDTH, 3 * CONV_WIDTH, 3 * CONV_WIDTH + SGU_WIDTH
        b_gate, c_gate, hc = proj[..., :c0], proj[..., c0:c1], proj[..., c1:c2]
        z = jax.nn.gelu(proj[..., c2:], approximate=False)
        u, v = z[..., :SGU_WIDTH], z[..., SGU_WIDTH:]
        y_conv = short_conv_mixer(b_gate, c_gate, hc, conv_w[l])
        y_sgu = spatial_gating_mixer(u, v, sgu_w[l], sgu_b[l], sgu_norm_g[l])
        y = jnp.concatenate([group_rmsnorm(y_conv, out_norm_conv_g[l], CONV_GROUPS),
                             group_rmsnorm(y_sgu, out_norm_sgu_g[l], SGU_GROUPS)], axis=-1)
        h = h + y @ w_out[l]
        hn = rmsnorm(h, ffn_norm_g[l])
        h = h + peer_ffn(hn, peer_w_q[l], peer_sub_keys[l], peer_u[l], peer_v[l])
    return rmsnorm(h, final_norm_g)
```

```python
import os
from contextlib import ExitStack

import numpy as np
import concourse.bass as bass
import concourse.mybir as mybir
from concourse.bass_utils import run_bass_kernel_spmd

F32 = mybir.dt.float32
BF16 = mybir.dt.bfloat16
AF = mybir.ActivationFunctionType
ALU = mybir.AluOpType
AX = mybir.AxisListType

NCORES = 8
SEQ = 16384
D = 1024
TOK = SEQ // NCORES
G = 256
NG = TOK // G
NT = TOK // 128
GW = G + 2
D_IN = 2560
NE = 16384
NCH = 128
N_ACT_HEADS = 3
EPS = 1e-6

ENGS = ("tensor", "vector", "scalar", "gpsimd", "sync")
SAME_ENG_WINDOW = 12


class Op:
    __slots__ = ("eng", "fn", "deps", "sig", "sem", "val", "idx", "dma", "lidx", "raw")

    def __init__(self, eng, fn, dma=None):
        self.eng = eng
        self.fn = fn
        self.deps = []
        self.sig = False
        self.sem = None
        self.val = 0
        self.dma = dma


class Prog:
    def __init__(self):
        self.ops = []
        self.last_w = {}
        self.readers = {}
        self.last_on_eng = {}
        self.pending_barrier = {}

    def op(self, eng, fn, reads=(), writes=(), dma=None):
        o = Op(eng, fn, dma)
        o.idx = len(self.ops)
        deps = set()
        for k in reads:
            w = self.last_w.get(k)
            if w is not None:
                deps.add(w)
        o.raw = set(deps)
        for k in writes:
            w = self.last_w.get(k)
            if w is not None:
                deps.add(w)
            for r in self.readers.get(k, ()):
                deps.add(r)
        if eng in self.pending_barrier:
            deps.update(self.pending_barrier.pop(eng))
        deps.discard(o.idx)
        o.deps = sorted(deps)
        for k in reads:
            self.readers.setdefault(k, []).append(o.idx)
        for k in writes:
            self.last_w[k] = o.idx
            self.readers[k] = []
        self.ops.append(o)
        self.last_on_eng[eng] = o.idx
        return o

    def barrier(self):
        lasts = list(self.last_on_eng.values())
        for e in ENGS:
            self.pending_barrier[e] = set(lasts)

    def emit(self, sems_eng, sems_dma):
        ops = self.ops
        lcnt = {e: 0 for e in ENGS}
        for o in ops:
            o.lidx = lcnt[o.eng]
            lcnt[o.eng] += 1
        for o in ops:
            for d in o.deps:
                p = ops[d]
                if p.dma is not None:
                    continue
                if p.eng == "tensor" and o.eng == "tensor":
                    continue
                if p.eng == o.eng and d not in o.raw and o.lidx - p.lidx >= SAME_ENG_WINDOW:
                    continue
                p.sig = True
        cnt = {e: 0 for e in ENGS}
        dcnt = {}
        for o in ops:
            if o.dma is not None:
                key, n = o.dma
                dcnt[key] = dcnt.get(key, 0) + 16 * n
                o.sem = sems_dma[key]
                o.val = dcnt[key]
            elif o.sig:
                cnt[o.eng] += 1
                o.sem = sems_eng[o.eng]
                o.val = cnt[o.eng]
        self.counts = (cnt, dcnt)
        per_eng = {e: [] for e in ENGS}
        for o in ops:
            per_eng[o.eng].append(o)
        return per_eng

    def run_engine(self, eng_name, eng, per_eng, final_waits=()):
        ops = self.ops
        waited = {}
        for o in per_eng[eng_name]:
            need = {}
            for d in o.deps:
                p = ops[d]
                if p.dma is None and p.eng == "tensor" and o.eng == "tensor":
                    continue
                if p.dma is None and p.eng == o.eng and d not in o.raw and o.lidx - p.lidx >= SAME_ENG_WINDOW:
                    continue
                s = p.sem
                key = id(s)
                if waited.get(key, 0) >= p.val:
                    continue
                if key not in need or need[key][1] < p.val:
                    need[key] = (s, p.val)
            for key, (s, v) in need.items():
                eng.wait_ge(s, v)
                waited[key] = v
            ins = o.fn(eng)
            if o.dma is None and o.sig:
                ins.then_inc(o.sem, 1)
        for s, v in final_waits:
            eng.wait_ge(s, v)


class Arena:
    def __init__(self, t, nwords):
        self.t = t
        self.n = nwords
        self.off = 0

    def reset(self):
        self.off = 0

    def f32(self, *shape):
        n = int(np.prod(shape))
        assert self.off + n <= self.n, ("arena overflow", self.off + n, self.n)
        v = self.t[:, self.off:self.off + n]
        self.off += (n + 7) // 8 * 8
        return self._shape(v, shape)

    def bf16(self, *shape):
        n = int(np.prod(shape))
        w = (n + 1) // 2
        w = (w + 7) // 8 * 8
        assert self.off + w <= self.n, ("arena overflow", self.off + w, self.n)
        v = self.t[:, self.off:self.off + w].bitcast(BF16)[:, 0:n]
        self.off += w
        return self._shape(v, shape)

    @staticmethod
    def _shape(v, shape):
        if len(shape) == 1:
            return v
        if len(shape) == 2:
            return v.rearrange("p (a b) -> p a b", a=shape[0])
        if len(shape) == 3:
            return v.rearrange("p (a b c) -> p a b c", a=shape[0], b=shape[1])
        raise ValueError(shape)


_DBG = {}


def build_nc(n_groups_b=NG, n_chunks=NCH, debug=False, stop=99):
    nc = bass.Bass("TRN2", target_bir_lowering=False)

    def din(name, shape):
        return nc.dram_tensor(name, list(shape), F32, kind="ExternalInput").ap()

    x_tm_d = din("x_tm", [TOK, D])
    xT_d = din("xT", [D, TOK + 2])
    w_in_d = din("w_in", [D, D_IN])
    w_out_d = din("w_out", [D, D])
    w_q_d = din("w_q", [D, 2048])
    keysT_d = din("keysT", [128, 16 * 128])
    UT_d = din("UT", [D, NE])
    V_d = din("V", [NE, D])
    cA_d = din("cA", [128, 8 + 8 + 12 + 4 + 4 + 4])
    cB_d = din("cB", [128, 128 + 128 + 128])
    wT_d = din("wT", [128, 512])
    Bb_d = din("Bb", [128, 512])
    gF_d = din("gF", [128, D])
    out_d = nc.dram_tensor("out", [TOK, D], F32, kind="ExternalOutput").ap()
    if debug:
        dbg_h_d = nc.dram_tensor("dbg_h", [TOK, D], F32, kind="ExternalOutput").ap()

    xT_v = xT_d.rearrange("(k p) t -> p k t", p=128)
    w_in_v = w_in_d.rearrange("(k p) c -> p k c", p=128)
    w_out_v = w_out_d.rearrange("(k p) c -> p k c", p=128)
    w_q_v = w_q_d.rearrange("(k p) c -> p k c", p=128)
    UT_v = UT_d.rearrange("(k p) e -> p k e", p=128)

    with ExitStack() as es:
        def sb(name, shape, dt):
            return es.enter_context(nc.sbuf_tensor(name, list(shape), dt))

        def ps(name, shape, dt):
            return es.enter_context(nc.psum_tensor(name, list(shape), dt))

        h_all = sb("h_all", [128, NT, D], F32)
        keys_bf = sb("keys_bf", [128, 16, 128], BF16)
        cA = sb("cA_sb", [128, 40], F32)
        cB = sb("cB_sb", [128, 384], F32)
        ident_bf = sb("ident_bf", [128, 128], BF16)
        wT_bf = sb("wT_bf", [128, 4, 128], BF16)
        Bb = sb("Bb_sb", [128, 4, 128], F32)
        gF = sb("gF_sb", [128, D], F32)
        ARENA_WORDS = 33000
        arena_t = sb("arena", [128, ARENA_WORDS], F32)
        AR = Arena(arena_t, ARENA_WORDS)

        g1 = cA[:, 0:8]
        g2 = cA[:, 8:16]
        cw = cA[:, 16:28].rearrange("p (m k) -> p m k", m=4)
        gv = cA[:, 28:32]
        gc = cA[:, 32:36]
        gs = cA[:, 36:40]
        ones32 = cB[:, 128:256]
        blk64 = cB[:, 256:384]

        pb = [ps("pb%d" % i, [128, 512], F32) for i in range(8)]
        pbT = pb[5][:].bitcast(BF16)
        bq = [pb[6][:].bitcast(BF16), pb[7][:].bitcast(BF16)]

        sems_eng = {e: es.enter_context(nc.semaphore("s_" + e)) for e in ENGS}
        sems_dma = {}

        def dsem(key):
            if key not in sems_dma:
                sems_dma[key] = es.enter_context(nc.semaphore("d_" + key))
            return sems_dma[key]

        P = Prog()

        def dma(eng, key, pairs, reads=(), writes=()):
            s = dsem(key)

            def fn(e):
                last = None
                for (o, i) in pairs:
                    last = e.dma_start(out=o, in_=i).then_inc(s, 16)
                return last
            return P.op(eng, fn, reads, writes, dma=(key, len(pairs)))

        V_ = lambda fn, r, w: P.op("vector", fn, r, w)
        S_ = lambda fn, r, w: P.op("scalar", fn, r, w)
        T_ = lambda fn, r, w: P.op("tensor", fn, r, w)

        def mm_group(out_ap, pairs, reads, writes):
            def fn(e):
                last = None
                n = len(pairs)
                for i, (l, r) in enumerate(pairs):
                    last = e.matmul(out_ap, lhsT=l, rhs=r, start=(i == 0), stop=(i == n - 1))
                return last
            return T_(fn, reads, writes)

        dma("sync", "consts", [(cA[:], cA_d), (cB[:], cB_d), (Bb[:].rearrange("p a b -> p (a b)"), Bb_d),
                               (gF[:], gF_d)], writes=["cA", "cB", "Bb", "gF"])
        dma("gpsimd", "keys", [(keys_bf[:].rearrange("p a b -> p (a b)"), keysT_d),
                               (wT_bf[:].rearrange("p a b -> p (a b)"), wT_d)], writes=["keys", "wT"])
        V_(lambda e: e.tensor_copy(out=ident_bf[:], in_=cB[:, 0:128]), ["cB"], ["ident"])
        V_(lambda e: e.memset(wT_bf[64:128, :, 0:64], 0.0), ["wT"], ["wT"])

        AR.reset()
        w_in_bf = AR.bf16(8, D_IN)
        w_out_bf = AR.bf16(8, D)
        xT32 = [AR.f32(8, GW) for _ in range(2)]
        sq32 = [AR.f32(GW) for _ in range(2)]
        sd = AR.f32(GW)
        Rb = AR.f32(GW)
        xnT = AR.bf16(8, GW)
        hc_sb = [AR.f32(GW) for _ in range(2)]
        zc = [AR.f32(GW) for _ in range(2)]
        t1 = AR.f32(G)
        t2 = AR.f32(G)
        t3 = AR.f32(G)
        yc = [AR.f32(G) for _ in range(4)]
        sqy = [AR.f32(G) for _ in range(2)]
        sdy = AR.f32(G)
        rsy = AR.f32(G)
        uT = [AR.f32(GW) for _ in range(4)]
        ynT = [AR.bf16(G) for _ in range(8)]
        v_sb = AR.f32(512)
        vjunk = AR.f32(128)
        ssv = AR.f32(4)
        sdv = AR.f32(4)
        rsv = AR.f32(4)
        v_bf = AR.bf16(4, 128)
        mixed = [AR.f32(128) for _ in range(2)]
        ys = [AR.f32(G) for _ in range(4)]

        dma("gpsimd", "w_in", [(w_in_bf[:], w_in_v)], writes=["w_in"])
        dma("gpsimd", "w_out", [(w_out_bf[:], w_out_v)], writes=["w_out"])

        bank_rr = [0]

        def next_bank():
            b = bank_rr[0] % 4
            bank_rr[0] += 1
            return b

        for gi in range(NG):
            xs = gi % 2
            c0 = gi * G
            dma("sync", "xT%d" % xs, [(xT32[xs][:], xT_v[:, :, c0:c0 + GW])],
                writes=["xT32_%d" % xs])
            dma("sync", "xtm%d" % gi, [(h_all[:, 2 * gi + tt, :], x_tm_d[(2 * gi + tt) * 128:(2 * gi + tt + 1) * 128, :])
                                        for tt in range(2)], writes=["h%d" % (2 * gi), "h%d" % (2 * gi + 1)])
            xk = "xT32_%d" % xs
            X = xT32[xs]
            for k in range(8):
                q = k % 2
                S_(lambda e, k=k, q=q, X=X: e.activation(out=sq32[q][:], in_=X[:, k, :], func=AF.Square),
                   [xk], ["sq32_%d" % q])
                T_(lambda e, k=k, q=q: e.matmul(pb[6][:, 0:GW], lhsT=ones32, rhs=sq32[q][:], start=(k == 0), stop=(k == 7)),
                   ["sq32_%d" % q, "cB"], ["pb6"])
            S_(lambda e: e.activation(out=sd[:], in_=pb[6][:, 0:GW], func=AF.Sqrt, bias=EPS, scale=1.0 / D), ["pb6"], ["sd"])
            V_(lambda e: e.reciprocal(out=Rb[:], in_=sd[:]), ["sd"], ["Rb"])
            for k in range(8):
                V_(lambda e, k=k, X=X: e.scalar_tensor_tensor(out=xnT[:, k, :], in0=X[:, k, :], scalar=g1[:, k:k + 1], in1=Rb[:],
                                                         op0=ALU.mult, op1=ALU.mult),
                   [xk, "Rb", "cA"], ["xnT%d" % k])
            xn_keys = ["xnT%d" % k for k in range(8)]

            def proj(col0, width=GW):
                b = next_bank()
                mm_group(pb[b][:, 0:width],
                         [(w_in_bf[:, k, col0:col0 + 128], xnT[:, k, 0:width]) for k in range(8)],
                         xn_keys + ["w_in"], ["pb%d" % b])
                return b

            for m in range(4):
                s2 = m % 2
                bh = proj(1024 + m * 128)
                S_(lambda e, bh=bh, s2=s2: e.copy(out=hc_sb[s2][:], in_=pb[bh][:, 0:GW]), ["pb%d" % bh], ["hc%d" % s2])
                bc = proj(512 + m * 128)
                V_(lambda e, bc=bc, s2=s2: e.tensor_tensor(out=zc[s2][:], in0=pb[bc][:, 0:GW], in1=hc_sb[s2][:], op=ALU.mult),
                   ["pb%d" % bc, "hc%d" % s2], ["zc%d" % s2])
                V_(lambda e, m=m, s2=s2: e.tensor_scalar(out=t1[:], in0=zc[s2][:, 0:G], scalar1=cw[:, m, 0:1], scalar2=None,
                                                          op0=ALU.mult), ["zc%d" % s2, "cA"], ["t1"])
                V_(lambda e, m=m, s2=s2: e.scalar_tensor_tensor(out=t2[:], in0=zc[s2][:, 1:G + 1], scalar=cw[:, m, 1:2], in1=t1[:],
                                                                 op0=ALU.mult, op1=ALU.add), ["zc%d" % s2, "t1", "cA"], ["t2"])
                V_(lambda e, m=m, s2=s2: e.scalar_tensor_tensor(out=t3[:], in0=zc[s2][:, 2:G + 2], scalar=cw[:, m, 2:3], in1=t2[:],
                                                                 op0=ALU.mult, op1=ALU.add), ["zc%d" % s2, "t2", "cA"], ["t3"])
                bb = proj(m * 128)
                V_(lambda e, bb=bb, m=m: e.tensor_tensor(out=yc[m][:], in0=pb[bb][:, 2:G + 2], in1=t3[:], op=ALU.mult),
                   ["pb%d" % bb, "t3"], ["yc%d" % m])
                S_(lambda e, m=m, s2=s2: e.activation(out=sqy[s2][:], in_=yc[m][:], func=AF.Square), ["yc%d" % m], ["sqy%d" % s2])
                T_(lambda e, s2=s2: e.matmul(pb[5][:, 0:G], lhsT=blk64, rhs=sqy[s2][:], start=True, stop=True),
                   ["sqy%d" % s2, "cB"], ["pb5"])
                S_(lambda e: e.activation(out=sdy[:], in_=pb[5][:, 0:G], func=AF.Sqrt, bias=EPS, scale=1.0 / 64), ["pb5"], ["sdy"])
                V_(lambda e: e.reciprocal(out=rsy[:], in_=sdy[:]), ["sdy"], ["rsy"])
                V_(lambda e, m=m: e.scalar_tensor_tensor(out=ynT[m][:], in0=yc[m][:], scalar=gc[:, m:m + 1], in1=rsy[:],
                                                         op0=ALU.mult, op1=ALU.mult), ["yc%d" % m, "rsy", "cA"], ["ynT%d" % m])
            for m in range(4):
                bu = proj(1536 + m * 128)
                S_(lambda e, bu=bu, m=m: e.activation(out=uT[m][:], in_=pb[bu][:, 0:GW], func=AF.Gelu), ["pb%d" % bu], ["uT%d" % m])
            for tt in range(2):
                cs = 2 + tt * 128
                mm_group(pb[4][:, 0:512], [(xnT[:, k, cs:cs + 128], w_in_bf[:, k, 2048:2560]) for k in range(8)],
                         xn_keys + ["w_in"], ["pb4"])
                S_(lambda e: e.activation(out=v_sb[:], in_=pb[4][:, 0:512], func=AF.Gelu), ["pb4"], ["v_sb"])
                for g in range(4):
                    S_(lambda e, g=g: e.activation(out=vjunk[:], in_=v_sb[:, g * 128:(g + 1) * 128], func=AF.Square,
                                                   accum_out=ssv[:, g:g + 1]), ["v_sb"], ["vjunk", "ssv%d" % g])
                S_(lambda e: e.activation(out=sdv[:], in_=ssv[:], func=AF.Sqrt, bias=EPS, scale=1.0 / 128),
                   ["ssv%d" % g for g in range(4)], ["sdv"])
                V_(lambda e: e.reciprocal(out=rsv[:], in_=sdv[:]), ["sdv"], ["rsv"])
                V_(lambda e: e.tensor_tensor(out=v_bf[:], in0=v_sb[:].rearrange("p (a b) -> p a b", a=4),
                                             in1=rsv[:].unsqueeze(2).to_broadcast([128, 4, 128]), op=ALU.mult),
                   ["v_sb", "rsv"], ["v_bf"])

                def mixfn(e):
                    last = None
                    for g in range(4):
                        last = e.matmul(pb[6][:, g * 128:(g + 1) * 128], lhsT=v_bf[:, g, :], rhs=wT_bf[:, g, :], start=True, stop=True)
                    return last
                T_(mixfn, ["v_bf", "wT"], ["pb6"])
                for g in range(4):
                    q = g % 2
                    V_(lambda e, g=g, q=q: e.scalar_tensor_tensor(out=mixed[q][:], in0=pb[6][:, g * 128:(g + 1) * 128],
                                                                  scalar=gv[:, g:g + 1], in1=Bb[:, g, :], op0=ALU.mult, op1=ALU.add),
                       ["pb6", "cA", "Bb"], ["mixed%d" % q])
                    V_(lambda e, g=g, q=q, cs=cs, tt=tt: e.tensor_tensor(out=ys[g][:, tt * 128:(tt + 1) * 128],
                                                                         in0=uT[g][:, cs:cs + 128], in1=mixed[q][:], op=ALU.mult),
                       ["uT%d" % g, "mixed%d" % q], ["ys%d_%d" % (g, tt)])
            for g in range(4):
                s2 = g % 2
                S_(lambda e, g=g, s2=s2: e.activation(out=sqy[s2][:], in_=ys[g][:], func=AF.Square),
                   ["ys%d_0" % g, "ys%d_1" % g], ["sqy%d" % s2])
                T_(lambda e, s2=s2: e.matmul(pb[5][:, 0:G], lhsT=ones32, rhs=sqy[s2][:], start=True, stop=True),
                   ["sqy%d" % s2, "cB"], ["pb5"])
                S_(lambda e: e.activation(out=sdy[:], in_=pb[5][:, 0:G], func=AF.Sqrt, bias=EPS, scale=1.0 / 128), ["pb5"], ["sdy"])
                V_(lambda e: e.reciprocal(out=rsy[:], in_=sdy[:]), ["sdy"], ["rsy"])
                V_(lambda e, g=g: e.scalar_tensor_tensor(out=ynT[4 + g][:], in0=ys[g][:], scalar=gs[:, g:g + 1], in1=rsy[:],
                                                         op0=ALU.mult, op1=ALU.mult),
                   ["ys%d_0" % g, "ys%d_1" % g, "rsy", "cA"], ["ynT%d" % (4 + g)])
            yn_keys = ["ynT%d" % m for m in range(8)]
            for tt in range(2):
                T = 2 * gi + tt
                for half in range(2):
                    b = next_bank()
                    mm_group(pb[b][:, 0:512],
                             [(ynT[m][:, tt * 128:(tt + 1) * 128], w_out_bf[:, m, half * 512:(half + 1) * 512]) for m in range(8)],
                             yn_keys + ["w_out"], ["pb%d" % b])
                    V_(lambda e, b=b, T=T, half=half: e.tensor_tensor(out=h_all[:, T, half * 512:(half + 1) * 512], in0=pb[b][:, 0:512],
                                                                      in1=h_all[:, T, half * 512:(half + 1) * 512], op=ALU.add),
                       ["pb%d" % b, "h%d" % T], ["h%d" % T])

        if debug:
            dma("sync", "dbg", [(dbg_h_d[T * 128:(T + 1) * 128, :], h_all[:, T, :]) for T in range(NT)],
                reads=["h%d" % T for T in range(NT)])

        P.barrier()
        AR.reset()
        wq_bf = AR.bf16(8, 2048)
        NSL = 2
        UT_bf = [AR.bf16(8, 256) for _ in range(NSL)]
        V_bf = [AR.bf16(2, D) for _ in range(NSL)]
        hn_tm = [AR.bf16(D) for _ in range(2)]
        hnT = AR.bf16(8, G)
        qT = [AR.bf16(G) for _ in range(16)]
        ab = [AR.f32(16, 128) for _ in range(2)]
        ssh = AR.f32(2)
        sdh = AR.f32(2)
        r2 = AR.f32(2)
        negm = [AR.f32(16) for _ in range(2)]
        top = [AR.f32(16, 16) for _ in range(2)]
        tmpa = [AR.f32(128) for _ in range(4)]
        cand = [AR.f32(16, 16) for _ in range(4)]
        tmpc = [AR.f32(256) for _ in range(4)]
        c16 = [AR.f32(8, 16) for _ in range(2)]
        Zs = AR.f32(8)
        kap = AR.f32(8)
        tauadj = [AR.f32(8) for _ in range(2)]
        Dm = [[AR.bf16(128) for _ in range(8)] for _ in range(4)]
        theta = [AR.f32(8, 128) for _ in range(2)]
        fb = [AR.bf16(8, 128) for _ in range(4)]
        GA = [AR.bf16(G) for _ in range(2)]
        PT = [AR.bf16(G) for _ in range(2)]
        outsb = [AR.f32(D) for _ in range(2)]
        ss3 = AR.f32(2)
        sd3 = AR.f32(2)
        r3 = AR.f32(2)

        dma("gpsimd", "wq", [(wq_bf[:], w_q_v)], writes=["wq"])
        _DBG.update(dict(ab=ab, top=top, c16=c16, negm=negm, kap=kap, Zs=Zs, GA=GA, r2=r2, outsb=outsb))

        ngb = n_groups_b
        for gi in range(ngb):
            for tt in range(2):
                T = 2 * gi + tt
                S_(lambda e, T=T, tt=tt: e.activation(out=outsb[tt][:], in_=h_all[:, T, :], func=AF.Square, accum_out=ssh[:, tt:tt + 1]),
                   ["h%d" % T], ["outsb%d" % tt, "ssh%d" % tt])
                S_(lambda e, tt=tt: e.activation(out=sdh[:, tt:tt + 1], in_=ssh[:, tt:tt + 1], func=AF.Sqrt, bias=EPS, scale=1.0 / D),
                   ["ssh%d" % tt], ["sdh%d" % tt])
                V_(lambda e, tt=tt: e.reciprocal(out=r2[:, tt:tt + 1], in_=sdh[:, tt:tt + 1]), ["sdh%d" % tt], ["r2_%d" % tt])
                S_(lambda e, T=T, tt=tt: e.activation(out=hn_tm[tt][:], in_=h_all[:, T, :], func=AF.Copy, scale=r2[:, tt:tt + 1]),
                   ["h%d" % T, "r2_%d" % tt], ["hn_tm%d" % tt])

                def trfn(e, tt=tt):
                    last = None
                    for k in range(8):
                        last = e.transpose(out=pbT[:, k * 128:(k + 1) * 128], in_=hn_tm[tt][:, k * 128:(k + 1) * 128], identity=ident_bf[:])
                    return last
                T_(trfn, ["hn_tm%d" % tt, "ident"], ["pb5"])
                V_(lambda e, tt=tt: e.tensor_tensor(out=hnT[:, :, tt * 128:(tt + 1) * 128], in0=pbT.rearrange("p (a b) -> p a b", a=8),
                                                    in1=g2.unsqueeze(2).to_broadcast([128, 8, 128]), op=ALU.mult),
                   ["pb5", "cA"], ["hnT_%d" % tt])
            hk = ["hnT_0", "hnT_1"]
            if stop <= 1:
                continue
            for hp in range(16):
                q = 4 + hp % 2
                mm_group(pb[q][:, 0:G], [(wq_bf[:, k, hp * 128:(hp + 1) * 128], hnT[:, k, :]) for k in range(8)],
                         hk + ["wq"], ["pb%d" % q])
                S_(lambda e, hp=hp, q=q: e.copy(out=qT[hp][:], in_=pb[q][:, 0:G]), ["pb%d" % q], ["qT%d" % hp])
            if stop <= 2:
                continue
            for tt in range(2):
                for q4 in range(4):
                    bk = 4 + (q4 % 2)

                    def scfn(e, tt=tt, q4=q4, bk=bk):
                        last = None
                        for j in range(4):
                            hp = q4 * 4 + j
                            last = e.matmul(pb[bk][:, j * 128:(j + 1) * 128], lhsT=qT[hp][:, tt * 128:(tt + 1) * 128], rhs=keys_bf[:, hp, :],
                                            start=True, stop=True)
                        return last
                    bkeys = ["pb%d" % bk]
                    T_(scfn, ["qT%d" % (q4 * 4 + j) for j in range(4)] + ["keys"], bkeys)
                    S_(lambda e, tt=tt, q4=q4, bk=bk: e.copy(out=ab[tt][:, q4 * 4:(q4 + 1) * 4, :].rearrange("p a b -> p (a b)"), in_=pb[bk][:, 0:512]),
                       bkeys, ["ab%d_%d" % (tt, q4 * 4 + j) for j in range(4)])
            if stop <= 3:
                continue
            for tt in range(2):
                abk = ["ab%d_%d" % (tt, hp) for hp in range(16)]
                V_(lambda e, tt=tt: e.tensor_reduce(out=negm[tt][:], in_=ab[tt][:], axis=AX.X, op=ALU.max, negate=True), abk, ["negm%d" % tt])
                for hp in range(16):
                    S_(lambda e, tt=tt, hp=hp: e.activation(out=ab[tt][:, hp, :], in_=ab[tt][:, hp, :], func=AF.Exp, bias=negm[tt][:, hp:hp + 1], scale=1.0),
                       ["ab%d_%d" % (tt, hp), "negm%d" % tt], ["ab%d_%d" % (tt, hp)])
                for hp in range(16):
                    V_(lambda e, tt=tt, hp=hp: e.max(out=top[tt][:, hp, 0:8], in_=ab[tt][:, hp, :]), ["ab%d_%d" % (tt, hp)], ["top%d_%d" % (tt, hp)])
                for q4 in range(4):
                    for hp in range(4 * q4, 4 * q4 + 4):
                        V_(lambda e, tt=tt, hp=hp: e.match_replace(out=tmpa[hp % 4][:], in_to_replace=top[tt][:, hp, 0:8], in_values=ab[tt][:, hp, :], imm_value=-1.0),
                           ["ab%d_%d" % (tt, hp), "top%d_%d" % (tt, hp)], ["tmpa%d" % (hp % 4)])
                    for hp in range(4 * q4, 4 * q4 + 4):
                        V_(lambda e, tt=tt, hp=hp: e.max(out=top[tt][:, hp, 8:16], in_=tmpa[hp % 4][:]), ["tmpa%d" % (hp % 4)], ["top%d_%d" % (tt, hp)])
                for q4 in range(2):
                    hs = range(4 * q4, 4 * q4 + 4)
                    for h in hs:
                        V_(lambda e, tt=tt, h=h: e.tensor_tensor(out=cand[h % 4][:], in0=top[tt][:, 2 * h, :].unsqueeze(2).to_broadcast([128, 16, 16]),
                                                                 in1=top[tt][:, 2 * h + 1, :].unsqueeze(1).to_broadcast([128, 16, 16]), op=ALU.mult),
                           ["top%d_%d" % (tt, 2 * h), "top%d_%d" % (tt, 2 * h + 1)], ["cand%d" % (h % 4)])
                    for h in hs:
                        V_(lambda e, tt=tt, h=h: e.max(out=c16[tt][:, h, 0:8], in_=cand[h % 4][:].rearrange("p a b -> p (a b)")),
                           ["cand%d" % (h % 4)], ["c16_%d_%d" % (tt, h)])
                    for h in hs:
                        V_(lambda e, tt=tt, h=h: e.match_replace(out=tmpc[h % 4][:], in_to_replace=c16[tt][:, h, 0:8],
                                                                 in_values=cand[h % 4][:].rearrange("p a b -> p (a b)"), imm_value=-1.0),
                           ["cand%d" % (h % 4), "c16_%d_%d" % (tt, h)], ["tmpc%d" % (h % 4)])
                    for h in hs:
                        V_(lambda e, tt=tt, h=h: e.max(out=c16[tt][:, h, 8:16], in_=tmpc[h % 4][:]), ["tmpc%d" % (h % 4)], ["c16_%d_%d" % (tt, h)])
                ck = ["c16_%d_%d" % (tt, h) for h in range(8)]
                V_(lambda e, tt=tt: e.tensor_reduce(out=Zs[:], in_=c16[tt][:], axis=AX.X, op=ALU.add), ck, ["Zs"])
                V_(lambda e: e.reciprocal(out=kap[:], in_=Zs[:]), ["Zs"], ["kap"])
                V_(lambda e, tt=tt: e.tensor_scalar(out=tauadj[tt][:], in0=c16[tt][:, :, 15], scalar1=1.0 - 2.0 ** -18, scalar2=None, op0=ALU.mult),
                   ck, ["tauadj%d" % tt])
                a_view = ab[tt][:].rearrange("p (h two) k -> p h two k", two=2)[:, :, 0, :]
                V_(lambda e, tt=tt, a_view=a_view: e.reciprocal(out=theta[tt][:], in_=a_view), abk + ck, ["theta%d" % tt])
                for h in range(8):
                    P.op("gpsimd", lambda e, tt=tt, h=h: e.tensor_scalar(out=theta[tt][:, h, :], in0=theta[tt][:, h, :], scalar1=tauadj[tt][:, h:h + 1],
                                                                         scalar2=1.0, op0=ALU.mult, op1=ALU.mult),
                         ["theta%d" % tt, "tauadj%d" % tt], ["theta%d" % tt])
                    P.op("gpsimd", lambda e, tt=tt, h=h: e.tensor_scalar(out=ab[tt][:, 2 * h, :], in0=ab[tt][:, 2 * h, :], scalar1=kap[:, h:h + 1],
                                                                         scalar2=1.0, op0=ALU.mult, op1=ALU.mult),
                         ["ab%d_%d" % (tt, 2 * h), "kap", "theta%d" % tt], ["ab%d_%d" % (tt, 2 * h)])
                b_view = ab[tt][:].rearrange("p (h two) k -> p h two k", two=2)[:, :, 1, :]
                S_(lambda e, tt=tt, b_view=b_view: e.activation(out=hn_tm[tt][:].rearrange("p (h k) -> p h k", h=8), in_=b_view, func=AF.Copy),
                   abk + ["hn_tm%d" % tt], ["hn_tm%d" % tt])

                pb4bf = pb[4][:].bitcast(BF16)
                bT = outsb[tt].bitcast(BF16)[:, 0:1024]

                def bt1fn(e, tt=tt, pb4bf=pb4bf):
                    last = None
                    for h in range(8):
                        last = e.transpose(out=pb4bf[:, h * 128:(h + 1) * 128], in_=hn_tm[tt][:, h * 128:(h + 1) * 128], identity=ident_bf[:])
                    return last
                T_(bt1fn, ["hn_tm%d" % tt, "ident"], ["pb4"])
                S_(lambda e, pb4bf=pb4bf, bT=bT: e.copy(out=bT, in_=pb4bf), ["pb4"], ["outsb%d" % tt])

                def bqfn(e, tt=tt, bT=bT):
                    last = None
                    for h in range(8):
                        last = e.transpose(out=bq[tt][:, h * 128:(h + 1) * 128], in_=bT[:, h * 128:(h + 1) * 128], identity=ident_bf[:])
                    return last
                T_(bqfn, ["outsb%d" % tt, "ident"], ["bq%d" % tt])
            if stop <= 4:
                continue

            def load_U(pc):
                sl = pc % NSL
                dma("gpsimd", "u%d" % sl, [(UT_bf[sl][:], UT_v[:, :, pc * 256:(pc + 1) * 256])], writes=["UT%d" % sl])

            def load_V(pc):
                sl = pc % NSL
                dma("gpsimd", "v%d" % sl, [(V_bf[sl][:], V_d[pc * 256:(pc + 1) * 256, :].rearrange("(c p) d -> p c d", p=128))],
                    writes=["Vb%d" % sl])

            def stage_A(c):
                sl = (c // 2) % NSL
                cc = c % 2
                s2 = c % 2
                mm_group(pb[4][:, 0:G], [(UT_bf[sl][:, k, cc * 128:(cc + 1) * 128], hnT[:, k, :]) for k in range(8)],
                         hk + ["UT%d" % sl], ["pb4"])
                S_(lambda e, s2=s2: e.activation(out=GA[s2][:], in_=pb[4][:, 0:G], func=AF.Gelu), ["pb4"], ["GA%d" % s2])

            def stage_D(c, tt):
                ds = (2 * c + tt) % 4
                for h in range(8):
                    rk = ["ab%d_%d" % (tt, 2 * h), "ident"]
                    if h < N_ACT_HEADS:
                        S_(lambda e, h=h, tt=tt, c=c, ds=ds: e.activation(out=Dm[ds][h][:], in_=ident_bf[:], func=AF.Copy,
                                                                       scale=ab[tt][:, 2 * h, c:c + 1]), rk, ["Dm%d_%d" % (ds, h)])
                    else:
                        P.op("gpsimd", lambda e, h=h, tt=tt, c=c, ds=ds: e.tensor_scalar(out=Dm[ds][h][:], in0=ident_bf[:],
                                                                                     scalar1=ab[tt][:, 2 * h, c:c + 1], scalar2=1.0,
                                                                                     op0=ALU.mult, op1=ALU.mult), rk, ["Dm%d_%d" % (ds, h)])

            def stage_M(c, tt):
                fs = (2 * c + tt) % 4
                for h in range(8):
                    V_(lambda e, h=h, tt=tt, c=c, fs=fs: e.scalar_tensor_tensor(out=fb[fs][:, h, :], in0=ab[tt][:, 2 * h + 1, :],
                                                                              scalar=theta[tt][:, h, c:c + 1], in1=bq[tt][:, h * 128:(h + 1) * 128],
                                                                              op0=ALU.is_ge, op1=ALU.mult),
                       ["ab%d_%d" % (tt, 2 * h + 1), "theta%d" % tt, "bq%d" % tt], ["fb%d_%d" % (fs, h)])

            def stage_W(c, tt):
                fs = (2 * c + tt) % 4
                ds = fs
                mm_group(pb[5][:, tt * 128:(tt + 1) * 128], [(fb[fs][:, h, :], Dm[ds][h][:]) for h in range(8)],
                         ["fb%d_%d" % (fs, h) for h in range(8)] + ["Dm%d_%d" % (ds, h) for h in range(8)], ["pb5"])

            def stage_P(c):
                s2 = c % 2
                V_(lambda e, s2=s2: e.tensor_tensor(out=PT[s2][:], in0=pb[5][:, 0:G], in1=GA[s2][:], op=ALU.mult),
                   ["pb5", "GA%d" % s2], ["PT%d" % s2])

            def stage_O(c):
                sl = (c // 2) % NSL
                cc = c % 2
                s2 = c % 2

                def ofn(e):
                    last = None
                    for tt in range(2):
                        for half in range(2):
                            last = e.matmul(pb[tt * 2 + half][:, 0:512], lhsT=PT[s2][:, tt * 128:(tt + 1) * 128],
                                            rhs=V_bf[sl][:, cc, half * 512:(half + 1) * 512], start=(c == 0), stop=(c == n_chunks - 1))
                    return last
                T_(ofn, ["PT%d" % s2, "Vb%d" % sl], ["pb0", "pb1", "pb2", "pb3"])

            npairs = n_chunks // 2
            for p_ in range(min(NSL, npairs)):
                load_U(p_)
                load_V(p_)
            stage_D(0, 0)
            stage_D(0, 1)
            stage_A(0)
            stage_M(0, 0)
            stage_W(0, 0)
            stage_M(0, 1)
            stage_W(0, 1)
            for c in range(n_chunks):
                nx = c + 1 < n_chunks
                if nx:
                    stage_D(c + 1, 0)
                    stage_D(c + 1, 1)
                    stage_A(c + 1)
                    stage_M(c + 1, 0)
                stage_P(c)
                if nx:
                    stage_W(c + 1, 0)
                stage_O(c)
                if c % 2 == 1 and (c + 3) // 2 < npairs and (c + 3) // 2 >= NSL:
                    load_U((c + 3) // 2)
                if c % 2 == 0 and (c + 2) // 2 < npairs and (c + 2) // 2 >= NSL:
                    load_V((c + 2) // 2)
                if nx:
                    stage_M(c + 1, 1)
                    stage_W(c + 1, 1)

            for tt in range(2):
                T = 2 * gi + tt
                for half in range(2):
                    b = tt * 2 + half
                    V_(lambda e, b=b, T=T, half=half: e.tensor_tensor(out=h_all[:, T, half * 512:(half + 1) * 512], in0=pb[b][:, 0:512],
                                                                      in1=h_all[:, T, half * 512:(half + 1) * 512], op=ALU.add),
                       ["pb%d" % b, "h%d" % T], ["h%d" % T, "pb%d" % b])
                S_(lambda e, T=T, tt=tt: e.activation(out=outsb[tt][:], in_=h_all[:, T, :], func=AF.Square, accum_out=ss3[:, tt:tt + 1]),
                   ["h%d" % T], ["outsb%d" % tt, "ss3_%d" % tt])
                S_(lambda e, tt=tt: e.activation(out=sd3[:, tt:tt + 1], in_=ss3[:, tt:tt + 1], func=AF.Sqrt, bias=EPS, scale=1.0 / D),
                   ["ss3_%d" % tt], ["sd3_%d" % tt])
                V_(lambda e, tt=tt: e.reciprocal(out=r3[:, tt:tt + 1], in_=sd3[:, tt:tt + 1]), ["sd3_%d" % tt], ["r3_%d" % tt])
                V_(lambda e, T=T, tt=tt: e.scalar_tensor_tensor(out=outsb[tt][:], in0=h_all[:, T, :], scalar=r3[:, tt:tt + 1], in1=gF[:],
                                                                op0=ALU.mult, op1=ALU.mult),
                   ["h%d" % T, "r3_%d" % tt, "gF"], ["outsb%d" % tt])
                dma("sync", "st%d" % tt, [(out_d[T * 128:(T + 1) * 128, :], outsb[tt][:])], reads=["outsb%d" % tt])

        per_eng = P.emit(sems_eng, sems_dma)
        cnt, dcnt = P.counts
        finals = [(sems_dma[k], dcnt[k]) for k in dcnt if k.startswith("st") or k == "dbg" or (debug and k == "wq")]
        with nc.Block() as block:
            @block.sync
            def _(e):
                P.run_engine("sync", e, per_eng, final_waits=finals)

            @block.gpsimd
            def _(e):
                P.run_engine("gpsimd", e, per_eng)

            @block.vector
            def _(e):
                P.run_engine("vector", e, per_eng)

            @block.scalar
            def _(e):
                P.run_engine("scalar", e, per_eng)

            @block.tensor
            def _(e):
                P.run_engine("tensor", e, per_eng)
    return nc


def _prep_inputs(x, attn_norm_g, w_in, conv_w, sgu_w, sgu_b, sgu_norm_g, out_norm_conv_g,
                 out_norm_sgu_g, w_out, ffn_norm_g, peer_w_q, peer_sub_keys, peer_u, peer_v,
                 final_norm_g):
    f = lambda a: np.ascontiguousarray(np.asarray(a, dtype=np.float32))
    x = f(x)[0]
    col = lambda g: f(g).reshape(-1, 128).T
    cA = np.concatenate([
        col(attn_norm_g[0]), col(ffn_norm_g[0]),
        f(conv_w[0]).reshape(3, 4, 128).transpose(2, 1, 0).reshape(128, 12),
        col(sgu_norm_g[0]), col(out_norm_conv_g[0]), col(out_norm_sgu_g[0])], axis=1)
    blk = np.zeros((128, 128), np.float32)
    blk[:64, :64] = 1.0
    blk[64:, 64:] = 1.0
    cB = np.concatenate([np.eye(128, dtype=np.float32), np.ones((128, 128), np.float32), blk], axis=1)
    wT = f(np.asarray(sgu_w)[0].transpose(2, 0, 1)).reshape(128, 512)
    Bb = f(np.broadcast_to(np.asarray(sgu_b)[0][None], (128, 4, 128))).reshape(128, 512)
    gF = f(np.broadcast_to(np.asarray(final_norm_g)[None, :], (128, D)))
    keysT = f(np.asarray(peer_sub_keys)[0].transpose(3, 0, 1, 2)).reshape(128, 2048)
    UT = f(np.asarray(peer_u)[0].T)
    Vv = f(np.asarray(peer_v)[0])
    shared = {"w_in": f(w_in[0]), "w_out": f(w_out[0]), "w_q": f(peer_w_q[0]), "keysT": keysT, "UT": UT, "V": Vv,
              "cA": f(cA), "cB": cB, "wT": wT, "Bb": Bb, "gF": gF}
    in_maps = []
    for r in range(NCORES):
        xs = x[r * TOK:(r + 1) * TOK]
        xT = np.zeros((D, TOK + 2), np.float32)
        xT[:, 2:] = xs.T
        if r > 0:
            xT[:, 0:2] = x[r * TOK - 2:r * TOK].T
        m = dict(shared)
        m["x_tm"] = np.ascontiguousarray(xs)
        m["xT"] = xT
        in_maps.append(m)
    return in_maps


_NC_CACHE = {}
_DBG = {}


def kernel(**inputs):
    in_maps = _prep_inputs(**inputs)
    key = "full"
    if key not in _NC_CACHE:
        _NC_CACHE[key] = build_nc()
    nc = _NC_CACHE[key]
    res = run_bass_kernel_spmd(nc, in_maps, core_ids=list(range(NCORES)))
    out = np.concatenate([np.asarray(r["out"], dtype=np.float32) for r in res.results], axis=0)
    return out.reshape(1, SEQ, D)
```

```python
import os
from contextlib import ExitStack

import numpy as np
import concourse.bass as bass
import concourse.mybir as mybir
from concourse.bass_utils import run_bass_kernel_spmd

F32 = mybir.dt.float32
BF16 = mybir.dt.bfloat16
AF = mybir.ActivationFunctionType
ALU = mybir.AluOpType
AX = mybir.AxisListType

NCORES = 8
SEQ = 16384
D = 1024
TOK = SEQ // NCORES
G = 256
NG = TOK // G
NT = TOK // 128
GW = G + 2
D_IN = 2560
NE = 16384
NCH = 128
N_ACT_HEADS = 3
EPS = 1e-6

ENGS = ("tensor", "vector", "scalar", "gpsimd", "sync")
SAME_ENG_WINDOW = 12


class Op:
    __slots__ = ("eng", "fn", "deps", "sig", "sem", "val", "idx", "dma", "lidx", "raw")

    def __init__(self, eng, fn, dma=None):
        self.eng = eng
        self.fn = fn
        self.deps = []
        self.sig = False
        self.sem = None
        self.val = 0
        self.dma = dma


class Prog:
    def __init__(self):
        self.ops = []
        self.last_w = {}
        self.readers = {}
        self.last_on_eng = {}
        self.pending_barrier = {}

    def op(self, eng, fn, reads=(), writes=(), dma=None):
        o = Op(eng, fn, dma)
        o.idx = len(self.ops)
        deps = set()
        for k in reads:
            w = self.last_w.get(k)
            if w is not None:
                deps.add(w)
        o.raw = set(deps)
        for k in writes:
            w = self.last_w.get(k)
            if w is not None:
                deps.add(w)
            for r in self.readers.get(k, ()):
                deps.add(r)
        if eng in self.pending_barrier:
            deps.update(self.pending_barrier.pop(eng))
        deps.discard(o.idx)
        o.deps = sorted(deps)
        for k in reads:
            self.readers.setdefault(k, []).append(o.idx)
        for k in writes:
            self.last_w[k] = o.idx
            self.readers[k] = []
        self.ops.append(o)
        self.last_on_eng[eng] = o.idx
        return o

    def barrier(self):
        lasts = list(self.last_on_eng.values())
        for e in ENGS:
            self.pending_barrier[e] = set(lasts)

    def emit(self, sems_eng, sems_dma):
        ops = self.ops
        lcnt = {e: 0 for e in ENGS}
        for o in ops:
            o.lidx = lcnt[o.eng]
            lcnt[o.eng] += 1
        for o in ops:
            for d in o.deps:
                p = ops[d]
                if p.dma is not None:
                    continue
                if p.eng == "tensor" and o.eng == "tensor":
                    continue
                if p.eng == o.eng and d not in o.raw and o.lidx - p.lidx >= SAME_ENG_WINDOW:
                    continue
                p.sig = True
        cnt = {e: 0 for e in ENGS}
        dcnt = {}
        for o in ops:
            if o.dma is not None:
                key, n = o.dma
                dcnt[key] = dcnt.get(key, 0) + 16 * n
                o.sem = sems_dma[key]
                o.val = dcnt[key]
            elif o.sig:
                cnt[o.eng] += 1
                o.sem = sems_eng[o.eng]
                o.val = cnt[o.eng]
        self.counts = (cnt, dcnt)
        per_eng = {e: [] for e in ENGS}
        for o in ops:
            per_eng[o.eng].append(o)
        return per_eng

    def run_engine(self, eng_name, eng, per_eng, final_waits=()):
        ops = self.ops
        waited = {}
        for o in per_eng[eng_name]:
            need = {}
            for d in o.deps:
                p = ops[d]
                if p.dma is None and p.eng == "tensor" and o.eng == "tensor":
                    continue
                if p.dma is None and p.eng == o.eng and d not in o.raw and o.lidx - p.lidx >= SAME_ENG_WINDOW:
                    continue
                s = p.sem
                key = id(s)
                if waited.get(key, 0) >= p.val:
                    continue
                if key not in need or need[key][1] < p.val:
                    need[key] = (s, p.val)
            for key, (s, v) in need.items():
                eng.wait_ge(s, v)
                waited[key] = v
            ins = o.fn(eng)
            if o.dma is None and o.sig:
                ins.then_inc(o.sem, 1)
        for s, v in final_waits:
            eng.wait_ge(s, v)


class Arena:
    def __init__(self, t, nwords):
        self.t = t
        self.n = nwords
        self.off = 0

    def reset(self):
        self.off = 0

    def f32(self, *shape):
        n = int(np.prod(shape))
        assert self.off + n <= self.n, ("arena overflow", self.off + n, self.n)
        v = self.t[:, self.off:self.off + n]
        self.off += (n + 7) // 8 * 8
        return self._shape(v, shape)

    def bf16(self, *shape):
        n = int(np.prod(shape))
        w = (n + 1) // 2
        w = (w + 7) // 8 * 8
        assert self.off + w <= self.n, ("arena overflow", self.off + w, self.n)
        v = self.t[:, self.off:self.off + w].bitcast(BF16)[:, 0:n]
        self.off += w
        return self._shape(v, shape)

    @staticmethod
    def _shape(v, shape):
        if len(shape) == 1:
            return v
        if len(shape) == 2:
            return v.rearrange("p (a b) -> p a b", a=shape[0])
        if len(shape) == 3:
            return v.rearrange("p (a b c) -> p a b c", a=shape[0], b=shape[1])
        raise ValueError(shape)


_DBG = {}


def build_nc(n_groups_b=NG, n_chunks=NCH, debug=False, stop=99):
    nc = bass.Bass("TRN2", target_bir_lowering=False)

    def din(name, shape):
        return nc.dram_tensor(name, list(shape), F32, kind="ExternalInput").ap()

    x_tm_d = din("x_tm", [TOK, D])
    xT_d = din("xT", [D, TOK + 2])
    w_in_d = din("w_in", [D, D_IN])
    w_out_d = din("w_out", [D, D])
    w_q_d = din("w_q", [D, 2048])
    keysT_d = din("keysT", [128, 16 * 128])
    UT_d = din("UT", [D, NE])
    V_d = din("V", [NE, D])
    cA_d = din("cA", [128, 8 + 8 + 12 + 4 + 4 + 4])
    cB_d = din("cB", [128, 128 + 128 + 128])
    wT_d = din("wT", [128, 512])
    Bb_d = din("Bb", [128, 512])
    gF_d = din("gF", [128, D])
    out_d = nc.dram_tensor("out", [TOK, D], F32, kind="ExternalOutput").ap()
    if debug:
        dbg_h_d = nc.dram_tensor("dbg_h", [TOK, D], F32, kind="ExternalOutput").ap()

    xT_v = xT_d.rearrange("(k p) t -> p k t", p=128)
    w_in_v = w_in_d.rearrange("(k p) c -> p k c", p=128)
    w_out_v = w_out_d.rearrange("(k p) c -> p k c", p=128)
    w_q_v = w_q_d.rearrange("(k p) c -> p k c", p=128)
    UT_v = UT_d.rearrange("(k p) e -> p k e", p=128)

    with ExitStack() as es:
        def sb(name, shape, dt):
            return es.enter_context(nc.sbuf_tensor(name, list(shape), dt))

        def ps(name, shape, dt):
            return es.enter_context(nc.psum_tensor(name, list(shape), dt))

        h_all = sb("h_all", [128, NT, D], F32)
        keys_bf = sb("keys_bf", [128, 16, 128], BF16)
        cA = sb("cA_sb", [128, 40], F32)
        cB = sb("cB_sb", [128, 384], F32)
        ident_bf = sb("ident_bf", [128, 128], BF16)
        wT_bf = sb("wT_bf", [128, 4, 128], BF16)
        Bb = sb("Bb_sb", [128, 4, 128], F32)
        gF = sb("gF_sb", [128, D], F32)
        ARENA_WORDS = 33000
        arena_t = sb("arena", [128, ARENA_WORDS], F32)
        AR = Arena(arena_t, ARENA_WORDS)

        g1 = cA[:, 0:8]
        g2 = cA[:, 8:16]
        cw = cA[:, 16:28].rearrange("p (m k) -> p m k", m=4)
        gv = cA[:, 28:32]
        gc = cA[:, 32:36]
        gs = cA[:, 36:40]
        ones32 = cB[:, 128:256]
        blk64 = cB[:, 256:384]

        pb = [ps("pb%d" % i, [128, 512], F32) for i in range(8)]
        pbT = pb[5][:].bitcast(BF16)
        bq = [pb[6][:].bitcast(BF16), pb[7][:].bitcast(BF16)]

        sems_eng = {e: es.enter_context(nc.semaphore("s_" + e)) for e in ENGS}
        sems_dma = {}

        def dsem(key):
            if key not in sems_dma:
                sems_dma[key] = es.enter_context(nc.semaphore("d_" + key))
            return sems_dma[key]

        P = Prog()

        def dma(eng, key, pairs, reads=(), writes=()):
            s = dsem(key)

            def fn(e):
                last = None
                for (o, i) in pairs:
                    last = e.dma_start(out=o, in_=i).then_inc(s, 16)
                return last
            return P.op(eng, fn, reads, writes, dma=(key, len(pairs)))

        V_ = lambda fn, r, w: P.op("vector", fn, r, w)
        S_ = lambda fn, r, w: P.op("scalar", fn, r, w)
        T_ = lambda fn, r, w: P.op("tensor", fn, r, w)

        def mm_group(out_ap, pairs, reads, writes):
            def fn(e):
                last = None
                n = len(pairs)
                for i, (l, r) in enumerate(pairs):
                    last = e.matmul(out_ap, lhsT=l, rhs=r, start=(i == 0), stop=(i == n - 1))
                return last
            return T_(fn, reads, writes)

        dma("sync", "consts", [(cA[:], cA_d), (cB[:], cB_d), (Bb[:].rearrange("p a b -> p (a b)"), Bb_d),
                               (gF[:], gF_d)], writes=["cA", "cB", "Bb", "gF"])
        dma("gpsimd", "keys", [(keys_bf[:].rearrange("p a b -> p (a b)"), keysT_d),
                               (wT_bf[:].rearrange("p a b -> p (a b)"), wT_d)], writes=["keys", "wT"])
        V_(lambda e: e.tensor_copy(out=ident_bf[:], in_=cB[:, 0:128]), ["cB"], ["ident"])
        V_(lambda e: e.memset(wT_bf[64:128, :, 0:64], 0.0), ["wT"], ["wT"])

        AR.reset()
        w_in_bf = AR.bf16(8, D_IN)
        w_out_bf = AR.bf16(8, D)
        xT32 = [AR.f32(8, GW) for _ in range(2)]
        sq32 = [AR.f32(GW) for _ in range(2)]
        sd = AR.f32(GW)
        Rb = AR.f32(GW)
        xnT = AR.bf16(8, GW)
        hc_sb = [AR.f32(GW) for _ in range(2)]
        zc = [AR.f32(GW) for _ in range(2)]
        t1 = AR.f32(G)
        t2 = AR.f32(G)
        t3 = AR.f32(G)
        yc = [AR.f32(G) for _ in range(4)]
        sqy = [AR.f32(G) for _ in range(2)]
        sdy = AR.f32(G)
        rsy = AR.f32(G)
        uT = [AR.f32(GW) for _ in range(4)]
        ynT = [AR.bf16(G) for _ in range(8)]
        v_sb = AR.f32(512)
        vjunk = AR.f32(128)
        ssv = AR.f32(4)
        sdv = AR.f32(4)
        rsv = AR.f32(4)
        v_bf = AR.bf16(4, 128)
        mixed = [AR.f32(128) for _ in range(2)]
        ys = [AR.f32(G) for _ in range(4)]

        dma("gpsimd", "w_in", [(w_in_bf[:], w_in_v)], writes=["w_in"])
        dma("gpsimd", "w_out", [(w_out_bf[:], w_out_v)], writes=["w_out"])

        bank_rr = [0]

        def next_bank():
            b = bank_rr[0] % 4
            bank_rr[0] += 1
            return b

        for gi in range(NG):
            xs = gi % 2
            c0 = gi * G
            dma("sync", "xT%d" % xs, [(xT32[xs][:], xT_v[:, :, c0:c0 + GW])],
                writes=["xT32_%d" % xs])
            dma("sync", "xtm%d" % gi, [(h_all[:, 2 * gi + tt, :], x_tm_d[(2 * gi + tt) * 128:(2 * gi + tt + 1) * 128, :])
                                        for tt in range(2)], writes=["h%d" % (2 * gi), "h%d" % (2 * gi + 1)])
            xk = "xT32_%d" % xs
            X = xT32[xs]
            for k in range(8):
                q = k % 2
                S_(lambda e, k=k, q=q, X=X: e.activation(out=sq32[q][:], in_=X[:, k, :], func=AF.Square),
                   [xk], ["sq32_%d" % q])
                T_(lambda e, k=k, q=q: e.matmul(pb[6][:, 0:GW], lhsT=ones32, rhs=sq32[q][:], start=(k == 0), stop=(k == 7)),
                   ["sq32_%d" % q, "cB"], ["pb6"])
            S_(lambda e: e.activation(out=sd[:], in_=pb[6][:, 0:GW], func=AF.Sqrt, bias=EPS, scale=1.0 / D), ["pb6"], ["sd"])
            V_(lambda e: e.reciprocal(out=Rb[:], in_=sd[:]), ["sd"], ["Rb"])
            for k in range(8):
                V_(lambda e, k=k, X=X: e.scalar_tensor_tensor(out=xnT[:, k, :], in0=X[:, k, :], scalar=g1[:, k:k + 1], in1=Rb[:],
                                                         op0=ALU.mult, op1=ALU.mult),
                   [xk, "Rb", "cA"], ["xnT%d" % k])
            xn_keys = ["xnT%d" % k for k in range(8)]

            def proj(col0, width=GW):
                b = next_bank()
                mm_group(pb[b][:, 0:width],
                         [(w_in_bf[:, k, col0:col0 + 128], xnT[:, k, 0:width]) for k in range(8)],
                         xn_keys + ["w_in"], ["pb%d" % b])
                return b

            for m in range(4):
                s2 = m % 2
                bh = proj(1024 + m * 128)
                S_(lambda e, bh=bh, s2=s2: e.copy(out=hc_sb[s2][:], in_=pb[bh][:, 0:GW]), ["pb%d" % bh], ["hc%d" % s2])
                bc = proj(512 + m * 128)
                V_(lambda e, bc=bc, s2=s2: e.tensor_tensor(out=zc[s2][:], in0=pb[bc][:, 0:GW], in1=hc_sb[s2][:], op=ALU.mult),
                   ["pb%d" % bc, "hc%d" % s2], ["zc%d" % s2])
                V_(lambda e, m=m, s2=s2: e.tensor_scalar(out=t1[:], in0=zc[s2][:, 0:G], scalar1=cw[:, m, 0:1], scalar2=None,
                                                          op0=ALU.mult), ["zc%d" % s2, "cA"], ["t1"])
                V_(lambda e, m=m, s2=s2: e.scalar_tensor_tensor(out=t2[:], in0=zc[s2][:, 1:G + 1], scalar=cw[:, m, 1:2], in1=t1[:],
                                                                 op0=ALU.mult, op1=ALU.add), ["zc%d" % s2, "t1", "cA"], ["t2"])
                V_(lambda e, m=m, s2=s2: e.scalar_tensor_tensor(out=t3[:], in0=zc[s2][:, 2:G + 2], scalar=cw[:, m, 2:3], in1=t2[:],
                                                                 op0=ALU.mult, op1=ALU.add), ["zc%d" % s2, "t2", "cA"], ["t3"])
                bb = proj(m * 128)
                V_(lambda e, bb=bb, m=m: e.tensor_tensor(out=yc[m][:], in0=pb[bb][:, 2:G + 2], in1=t3[:], op=ALU.mult),
                   ["pb%d" % bb, "t3"], ["yc%d" % m])
                S_(lambda e, m=m, s2=s2: e.activation(out=sqy[s2][:], in_=yc[m][:], func=AF.Square), ["yc%d" % m], ["sqy%d" % s2])
                T_(lambda e, s2=s2: e.matmul(pb[5][:, 0:G], lhsT=blk64, rhs=sqy[s2][:], start=True, stop=True),
                   ["sqy%d" % s2, "cB"], ["pb5"])
                S_(lambda e: e.activation(out=sdy[:], in_=pb[5][:, 0:G], func=AF.Sqrt, bias=EPS, scale=1.0 / 64), ["pb5"], ["sdy"])
                V_(lambda e: e.reciprocal(out=rsy[:], in_=sdy[:]), ["sdy"], ["rsy"])
                V_(lambda e, m=m: e.scalar_tensor_tensor(out=ynT[m][:], in0=yc[m][:], scalar=gc[:, m:m + 1], in1=rsy[:],
                                                         op0=ALU.mult, op1=ALU.mult), ["yc%d" % m, "rsy", "cA"], ["ynT%d" % m])
            for m in range(4):
                bu = proj(1536 + m * 128)
                S_(lambda e, bu=bu, m=m: e.activation(out=uT[m][:], in_=pb[bu][:, 0:GW], func=AF.Gelu), ["pb%d" % bu], ["uT%d" % m])
            for tt in range(2):
                cs = 2 + tt * 128
                mm_group(pb[4][:, 0:512], [(xnT[:, k, cs:cs + 128], w_in_bf[:, k, 2048:2560]) for k in range(8)],
                         xn_keys + ["w_in"], ["pb4"])
                S_(lambda e: e.activation(out=v_sb[:], in_=pb[4][:, 0:512], func=AF.Gelu), ["pb4"], ["v_sb"])
                for g in range(4):
                    S_(lambda e, g=g: e.activation(out=vjunk[:], in_=v_sb[:, g * 128:(g + 1) * 128], func=AF.Square,
                                                   accum_out=ssv[:, g:g + 1]), ["v_sb"], ["vjunk", "ssv%d" % g])
                S_(lambda e: e.activation(out=sdv[:], in_=ssv[:], func=AF.Sqrt, bias=EPS, scale=1.0 / 128),
                   ["ssv%d" % g for g in range(4)], ["sdv"])
                V_(lambda e: e.reciprocal(out=rsv[:], in_=sdv[:]), ["sdv"], ["rsv"])
                V_(lambda e: e.tensor_tensor(out=v_bf[:], in0=v_sb[:].rearrange("p (a b) -> p a b", a=4),
                                             in1=rsv[:].unsqueeze(2).to_broadcast([128, 4, 128]), op=ALU.mult),
                   ["v_sb", "rsv"], ["v_bf"])

                def mixfn(e):
                    last = None
                    for g in range(4):
                        last = e.matmul(pb[6][:, g * 128:(g + 1) * 128], lhsT=v_bf[:, g, :], rhs=wT_bf[:, g, :], start=True, stop=True)
                    return last
                T_(mixfn, ["v_bf", "wT"], ["pb6"])
                for g in range(4):
                    q = g % 2
                    V_(lambda e, g=g, q=q: e.scalar_tensor_tensor(out=mixed[q][:], in0=pb[6][:, g * 128:(g + 1) * 128],
                                                                  scalar=gv[:, g:g + 1], in1=Bb[:, g, :], op0=ALU.mult, op1=ALU.add),
                       ["pb6", "cA", "Bb"], ["mixed%d" % q])
                    V_(lambda e, g=g, q=q, cs=cs, tt=tt: e.tensor_tensor(out=ys[g][:, tt * 128:(tt + 1) * 128],
                                                                         in0=uT[g][:, cs:cs + 128], in1=mixed[q][:], op=ALU.mult),
                       ["uT%d" % g, "mixed%d" % q], ["ys%d_%d" % (g, tt)])
            for g in range(4):
                s2 = g % 2
                S_(lambda e, g=g, s2=s2: e.activation(out=sqy[s2][:], in_=ys[g][:], func=AF.Square),
                   ["ys%d_0" % g, "ys%d_1" % g], ["sqy%d" % s2])
                T_(lambda e, s2=s2: e.matmul(pb[5][:, 0:G], lhsT=ones32, rhs=sqy[s2][:], start=True, stop=True),
                   ["sqy%d" % s2, "cB"], ["pb5"])
                S_(lambda e: e.activation(out=sdy[:], in_=pb[5][:, 0:G], func=AF.Sqrt, bias=EPS, scale=1.0 / 128), ["pb5"], ["sdy"])
                V_(lambda e: e.reciprocal(out=rsy[:], in_=sdy[:]), ["sdy"], ["rsy"])
                V_(lambda e, g=g: e.scalar_tensor_tensor(out=ynT[4 + g][:], in0=ys[g][:], scalar=gs[:, g:g + 1], in1=rsy[:],
                                                         op0=ALU.mult, op1=ALU.mult),
                   ["ys%d_0" % g, "ys%d_1" % g, "rsy", "cA"], ["ynT%d" % (4 + g)])
            yn_keys = ["ynT%d" % m for m in range(8)]
            for tt in range(2):
                T = 2 * gi + tt
                for half in range(2):
                    b = next_bank()
                    mm_group(pb[b][:, 0:512],
                             [(ynT[m][:, tt * 128:(tt + 1) * 128], w_out_bf[:, m, half * 512:(half + 1) * 512]) for m in range(8)],
                             yn_keys + ["w_out"], ["pb%d" % b])
                    V_(lambda e, b=b, T=T, half=half: e.tensor_tensor(out=h_all[:, T, half * 512:(half + 1) * 512], in0=pb[b][:, 0:512],
                                                                      in1=h_all[:, T, half * 512:(half + 1) * 512], op=ALU.add),
                       ["pb%d" % b, "h%d" % T], ["h%d" % T])

        if debug:
            dma("sync", "dbg", [(dbg_h_d[T * 128:(T + 1) * 128, :], h_all[:, T, :]) for T in range(NT)],
                reads=["h%d" % T for T in range(NT)])

        P.barrier()
        AR.reset()
        wq_bf = AR.bf16(8, 2048)
        NSL = 2
        UT_bf = [AR.bf16(8, 256) for _ in range(NSL)]
        V_bf = [AR.bf16(2, D) for _ in range(NSL)]
        hn_tm = [AR.bf16(D) for _ in range(2)]
        hnT = AR.bf16(8, G)
        qT = [AR.bf16(G) for _ in range(16)]
        ab = [AR.f32(16, 128) for _ in range(2)]
        ssh = AR.f32(2)
        sdh = AR.f32(2)
        r2 = AR.f32(2)
        negm = [AR.f32(16) for _ in range(2)]
        top = [AR.f32(16, 16) for _ in range(2)]
        tmpa = [AR.f32(128) for _ in range(4)]
        cand = [AR.f32(16, 16) for _ in range(4)]
        tmpc = [AR.f32(256) for _ in range(4)]
        c16 = [AR.f32(8, 16) for _ in range(2)]
        Zs = AR.f32(8)
        kap = AR.f32(8)
        tauadj = [AR.f32(8) for _ in range(2)]
        Dm = [[AR.bf16(128) for _ in range(8)] for _ in range(4)]
        theta = [AR.f32(8, 128) for _ in range(2)]
        fb = [AR.bf16(8, 128) for _ in range(4)]
        GA = [AR.bf16(G) for _ in range(2)]
        PT = [AR.bf16(G) for _ in range(2)]
        outsb = [AR.f32(D) for _ in range(2)]
        ss3 = AR.f32(2)
        sd3 = AR.f32(2)
        r3 = AR.f32(2)

        dma("gpsimd", "wq", [(wq_bf[:], w_q_v)], writes=["wq"])
        _DBG.update(dict(ab=ab, top=top, c16=c16, negm=negm, kap=kap, Zs=Zs, GA=GA, r2=r2, outsb=outsb))

        def end_group(gi):
            for tt in range(2):
                T = 2 * gi + tt
                for half in range(2):
                    b = tt * 2 + half
                    V_(lambda e, b=b, T=T, half=half: e.tensor_tensor(out=h_all[:, T, half * 512:(half + 1) * 512], in0=pb[b][:, 0:512],
                                                                      in1=h_all[:, T, half * 512:(half + 1) * 512], op=ALU.add),
                       ["pb%d" % b, "h%d" % T], ["h%d" % T, "pb%d" % b])
                S_(lambda e, T=T, tt=tt: e.activation(out=outsb[tt][:], in_=h_all[:, T, :], func=AF.Square, accum_out=ss3[:, tt:tt + 1]),
                   ["h%d" % T], ["outsb%d" % tt, "ss3_%d" % tt])
                S_(lambda e, tt=tt: e.activation(out=sd3[:, tt:tt + 1], in_=ss3[:, tt:tt + 1], func=AF.Sqrt, bias=EPS, scale=1.0 / D),
                   ["ss3_%d" % tt], ["sd3_%d" % tt])
                V_(lambda e, tt=tt: e.reciprocal(out=r3[:, tt:tt + 1], in_=sd3[:, tt:tt + 1]), ["sd3_%d" % tt], ["r3_%d" % tt])
                V_(lambda e, T=T, tt=tt: e.scalar_tensor_tensor(out=outsb[tt][:], in0=h_all[:, T, :], scalar=r3[:, tt:tt + 1], in1=gF[:],
                                                                op0=ALU.mult, op1=ALU.mult),
                   ["h%d" % T, "r3_%d" % tt, "gF"], ["outsb%d" % tt])
                dma("sync", "st%d" % tt, [(out_d[T * 128:(T + 1) * 128, :], outsb[tt][:])], reads=["outsb%d" % tt])

        ngb = n_groups_b
        for gi in range(ngb):
            for tt in range(2):
                T = 2 * gi + tt
                S_(lambda e, T=T, tt=tt: e.activation(out=outsb[tt][:], in_=h_all[:, T, :], func=AF.Square, accum_out=ssh[:, tt:tt + 1]),
                   ["h%d" % T], ["outsb%d" % tt, "ssh%d" % tt])
                S_(lambda e, tt=tt: e.activation(out=sdh[:, tt:tt + 1], in_=ssh[:, tt:tt + 1], func=AF.Sqrt, bias=EPS, scale=1.0 / D),
                   ["ssh%d" % tt], ["sdh%d" % tt])
                V_(lambda e, tt=tt: e.reciprocal(out=r2[:, tt:tt + 1], in_=sdh[:, tt:tt + 1]), ["sdh%d" % tt], ["r2_%d" % tt])
                S_(lambda e, T=T, tt=tt: e.activation(out=hn_tm[tt][:], in_=h_all[:, T, :], func=AF.Copy, scale=r2[:, tt:tt + 1]),
                   ["h%d" % T, "r2_%d" % tt], ["hn_tm%d" % tt])

                def trfn(e, tt=tt):
                    last = None
                    for k in range(8):
                        last = e.transpose(out=pbT[:, k * 128:(k + 1) * 128], in_=hn_tm[tt][:, k * 128:(k + 1) * 128], identity=ident_bf[:])
                    return last
                T_(trfn, ["hn_tm%d" % tt, "ident"], ["pb5"])
                V_(lambda e, tt=tt: e.tensor_tensor(out=hnT[:, :, tt * 128:(tt + 1) * 128], in0=pbT.rearrange("p (a b) -> p a b", a=8),
                                                    in1=g2.unsqueeze(2).to_broadcast([128, 8, 128]), op=ALU.mult),
                   ["pb5", "cA"], ["hnT_%d" % tt])
            hk = ["hnT_0", "hnT_1"]
            if stop <= 1:
                continue
            for hp in range(16):
                q = 4 + hp % 2
                mm_group(pb[q][:, 0:G], [(wq_bf[:, k, hp * 128:(hp + 1) * 128], hnT[:, k, :]) for k in range(8)],
                         hk + ["wq"], ["pb%d" % q])
                S_(lambda e, hp=hp, q=q: e.copy(out=qT[hp][:], in_=pb[q][:, 0:G]), ["pb%d" % q], ["qT%d" % hp])
            if stop <= 2:
                continue
            for tt in range(2):
                for q4 in range(4):
                    bk = 4 + (q4 % 2)

                    def scfn(e, tt=tt, q4=q4, bk=bk):
                        last = None
                        for j in range(4):
                            hp = q4 * 4 + j
                            last = e.matmul(pb[bk][:, j * 128:(j + 1) * 128], lhsT=qT[hp][:, tt * 128:(tt + 1) * 128], rhs=keys_bf[:, hp, :],
                                            start=True, stop=True)
                        return last
                    bkeys = ["pb%d" % bk]
                    T_(scfn, ["qT%d" % (q4 * 4 + j) for j in range(4)] + ["keys"], bkeys)
                    S_(lambda e, tt=tt, q4=q4, bk=bk: e.copy(out=ab[tt][:, q4 * 4:(q4 + 1) * 4, :].rearrange("p a b -> p (a b)"), in_=pb[bk][:, 0:512]),
                       bkeys, ["ab%d_%d" % (tt, q4 * 4 + j) for j in range(4)])
            if gi > 0:
                end_group(gi - 1)
            if stop <= 3:
                continue
            for tt in range(2):
                abk = ["ab%d_%d" % (tt, hp) for hp in range(16)]
                V_(lambda e, tt=tt: e.tensor_reduce(out=negm[tt][:], in_=ab[tt][:], axis=AX.X, op=ALU.max, negate=True), abk, ["negm%d" % tt])
                for hp in range(16):
                    S_(lambda e, tt=tt, hp=hp: e.activation(out=ab[tt][:, hp, :], in_=ab[tt][:, hp, :], func=AF.Exp, bias=negm[tt][:, hp:hp + 1], scale=1.0),
                       ["ab%d_%d" % (tt, hp), "negm%d" % tt], ["ab%d_%d" % (tt, hp)])
                for hp in range(16):
                    V_(lambda e, tt=tt, hp=hp: e.max(out=top[tt][:, hp, 0:8], in_=ab[tt][:, hp, :]), ["ab%d_%d" % (tt, hp)], ["top%d_%d" % (tt, hp)])
                for q4 in range(4):
                    for hp in range(4 * q4, 4 * q4 + 4):
                        V_(lambda e, tt=tt, hp=hp: e.match_replace(out=tmpa[hp % 4][:], in_to_replace=top[tt][:, hp, 0:8], in_values=ab[tt][:, hp, :], imm_value=-1.0),
                           ["ab%d_%d" % (tt, hp), "top%d_%d" % (tt, hp)], ["tmpa%d" % (hp % 4)])
                    for hp in range(4 * q4, 4 * q4 + 4):
                        V_(lambda e, tt=tt, hp=hp: e.max(out=top[tt][:, hp, 8:16], in_=tmpa[hp % 4][:]), ["tmpa%d" % (hp % 4)], ["top%d_%d" % (tt, hp)])
                for q4 in range(2):
                    hs = range(4 * q4, 4 * q4 + 4)
                    for h in hs:
                        V_(lambda e, tt=tt, h=h: e.tensor_tensor(out=cand[h % 4][:], in0=top[tt][:, 2 * h, :].unsqueeze(2).to_broadcast([128, 16, 16]),
                                                                 in1=top[tt][:, 2 * h + 1, :].unsqueeze(1).to_broadcast([128, 16, 16]), op=ALU.mult),
                           ["top%d_%d" % (tt, 2 * h), "top%d_%d" % (tt, 2 * h + 1)], ["cand%d" % (h % 4)])
                    for h in hs:
                        V_(lambda e, tt=tt, h=h: e.max(out=c16[tt][:, h, 0:8], in_=cand[h % 4][:].rearrange("p a b -> p (a b)")),
                           ["cand%d" % (h % 4)], ["c16_%d_%d" % (tt, h)])
                    for h in hs:
                        V_(lambda e, tt=tt, h=h: e.match_replace(out=tmpc[h % 4][:], in_to_replace=c16[tt][:, h, 0:8],
                                                                 in_values=cand[h % 4][:].rearrange("p a b -> p (a b)"), imm_value=-1.0),
                           ["cand%d" % (h % 4), "c16_%d_%d" % (tt, h)], ["tmpc%d" % (h % 4)])
                    for h in hs:
                        V_(lambda e, tt=tt, h=h: e.max(out=c16[tt][:, h, 8:16], in_=tmpc[h % 4][:]), ["tmpc%d" % (h % 4)], ["c16_%d_%d" % (tt, h)])
                ck = ["c16_%d_%d" % (tt, h) for h in range(8)]
                V_(lambda e, tt=tt: e.tensor_reduce(out=Zs[:], in_=c16[tt][:], axis=AX.X, op=ALU.add), ck, ["Zs"])
                V_(lambda e: e.reciprocal(out=kap[:], in_=Zs[:]), ["Zs"], ["kap"])
                V_(lambda e, tt=tt: e.tensor_scalar(out=tauadj[tt][:], in0=c16[tt][:, :, 15], scalar1=1.0 - 2.0 ** -18, scalar2=None, op0=ALU.mult),
                   ck, ["tauadj%d" % tt])
                a_view = ab[tt][:].rearrange("p (h two) k -> p h two k", two=2)[:, :, 0, :]
                V_(lambda e, tt=tt, a_view=a_view: e.reciprocal(out=theta[tt][:], in_=a_view), abk + ck, ["theta%d" % tt])
                for h in range(8):
                    V_(lambda e, tt=tt, h=h: e.tensor_scalar(out=theta[tt][:, h, :], in0=theta[tt][:, h, :], scalar1=tauadj[tt][:, h:h + 1],
                                                             scalar2=None, op0=ALU.mult), ["theta%d" % tt, "tauadj%d" % tt], ["theta%d" % tt])
                    V_(lambda e, tt=tt, h=h: e.tensor_scalar(out=ab[tt][:, 2 * h, :], in0=ab[tt][:, 2 * h, :], scalar1=kap[:, h:h + 1],
                                                             scalar2=None, op0=ALU.mult), ["ab%d_%d" % (tt, 2 * h), "kap", "theta%d" % tt],
                       ["ab%d_%d" % (tt, 2 * h)])
                b_view = ab[tt][:].rearrange("p (h two) k -> p h two k", two=2)[:, :, 1, :]
                S_(lambda e, tt=tt, b_view=b_view: e.activation(out=hn_tm[tt][:].rearrange("p (h k) -> p h k", h=8), in_=b_view, func=AF.Copy),
                   abk + ["hn_tm%d" % tt], ["hn_tm%d" % tt])

                pb4bf = pb[4][:].bitcast(BF16)
                bT = outsb[tt].bitcast(BF16)[:, 0:1024]

                def bt1fn(e, tt=tt, pb4bf=pb4bf):
                    last = None
                    for h in range(8):
                        last = e.transpose(out=pb4bf[:, h * 128:(h + 1) * 128], in_=hn_tm[tt][:, h * 128:(h + 1) * 128], identity=ident_bf[:])
                    return last
                T_(bt1fn, ["hn_tm%d" % tt, "ident"], ["pb4"])
                S_(lambda e, pb4bf=pb4bf, bT=bT: e.copy(out=bT, in_=pb4bf), ["pb4"], ["outsb%d" % tt])

                def bqfn(e, tt=tt, bT=bT):
                    last = None
                    for h in range(8):
                        last = e.transpose(out=bq[tt][:, h * 128:(h + 1) * 128], in_=bT[:, h * 128:(h + 1) * 128], identity=ident_bf[:])
                    return last
                T_(bqfn, ["outsb%d" % tt, "ident"], ["bq%d" % tt])
            if stop <= 4:
                continue

            def load_U(pc):
                sl = pc % NSL
                dma("gpsimd", "u%d" % sl, [(UT_bf[sl][:], UT_v[:, :, pc * 256:(pc + 1) * 256])], writes=["UT%d" % sl])

            def load_V(pc):
                sl = pc % NSL
                dma("gpsimd", "v%d" % sl, [(V_bf[sl][:], V_d[pc * 256:(pc + 1) * 256, :].rearrange("(c p) d -> p c d", p=128))],
                    writes=["Vb%d" % sl])

            def stage_A(c):
                sl = (c // 2) % NSL
                cc = c % 2
                s2 = c % 2
                mm_group(pb[4][:, 0:G], [(UT_bf[sl][:, k, cc * 128:(cc + 1) * 128], hnT[:, k, :]) for k in range(8)],
                         hk + ["UT%d" % sl], ["pb4"])
                S_(lambda e, s2=s2: e.activation(out=GA[s2][:], in_=pb[4][:, 0:G], func=AF.Gelu), ["pb4"], ["GA%d" % s2])

            def stage_D(c, tt):
                ds = (2 * c + tt) % 4
                for h in range(8):
                    rk = ["ab%d_%d" % (tt, 2 * h), "ident"]
                    if h < N_ACT_HEADS:
                        S_(lambda e, h=h, tt=tt, c=c, ds=ds: e.activation(out=Dm[ds][h][:], in_=ident_bf[:], func=AF.Copy,
                                                                       scale=ab[tt][:, 2 * h, c:c + 1]), rk, ["Dm%d_%d" % (ds, h)])
                    else:
                        P.op("gpsimd", lambda e, h=h, tt=tt, c=c, ds=ds: e.tensor_scalar(out=Dm[ds][h][:], in0=ident_bf[:],
                                                                                     scalar1=ab[tt][:, 2 * h, c:c + 1], scalar2=1.0,
                                                                                     op0=ALU.mult, op1=ALU.mult), rk, ["Dm%d_%d" % (ds, h)])

            def stage_M(c, tt):
                fs = (2 * c + tt) % 4
                for h in range(8):
                    V_(lambda e, h=h, tt=tt, c=c, fs=fs: e.scalar_tensor_tensor(out=fb[fs][:, h, :], in0=ab[tt][:, 2 * h + 1, :],
                                                                              scalar=theta[tt][:, h, c:c + 1], in1=bq[tt][:, h * 128:(h + 1) * 128],
                                                                              op0=ALU.is_ge, op1=ALU.mult),
                       ["ab%d_%d" % (tt, 2 * h + 1), "theta%d" % tt, "bq%d" % tt], ["fb%d_%d" % (fs, h)])

            def stage_W(c, tt):
                fs = (2 * c + tt) % 4
                ds = fs
                mm_group(pb[5][:, tt * 128:(tt + 1) * 128], [(fb[fs][:, h, :], Dm[ds][h][:]) for h in range(8)],
                         ["fb%d_%d" % (fs, h) for h in range(8)] + ["Dm%d_%d" % (ds, h) for h in range(8)], ["pb5"])

            def stage_P(c):
                s2 = c % 2
                V_(lambda e, s2=s2: e.tensor_tensor(out=PT[s2][:], in0=pb[5][:, 0:G], in1=GA[s2][:], op=ALU.mult),
                   ["pb5", "GA%d" % s2], ["PT%d" % s2])

            def stage_O(c):
                sl = (c // 2) % NSL
                cc = c % 2
                s2 = c % 2

                def ofn(e):
                    last = None
                    for tt in range(2):
                        for half in range(2):
                            last = e.matmul(pb[tt * 2 + half][:, 0:512], lhsT=PT[s2][:, tt * 128:(tt + 1) * 128],
                                            rhs=V_bf[sl][:, cc, half * 512:(half + 1) * 512], start=(c == 0), stop=(c == n_chunks - 1))
                    return last
                T_(ofn, ["PT%d" % s2, "Vb%d" % sl], ["pb0", "pb1", "pb2", "pb3"])

            npairs = n_chunks // 2
            for p_ in range(min(NSL, npairs)):
                load_U(p_)
                load_V(p_)
            stage_D(0, 0)
            stage_D(0, 1)
            stage_A(0)
            stage_M(0, 0)
            stage_W(0, 0)
            stage_M(0, 1)
            stage_W(0, 1)
            for c in range(n_chunks):
                nx = c + 1 < n_chunks
                if nx:
                    stage_D(c + 1, 0)
                    stage_D(c + 1, 1)
                    stage_A(c + 1)
                    stage_M(c + 1, 0)
                stage_P(c)
                if nx:
                    stage_W(c + 1, 0)
                stage_O(c)
                if c % 2 == 1 and (c + 3) // 2 < npairs and (c + 3) // 2 >= NSL:
                    load_U((c + 3) // 2)
                if c % 2 == 0 and (c + 2) // 2 < npairs and (c + 2) // 2 >= NSL:
                    load_V((c + 2) // 2)
                if nx:
                    stage_M(c + 1, 1)
                    stage_W(c + 1, 1)

        if ngb > 0 and stop == 99:
            end_group(ngb - 1)

        per_eng = P.emit(sems_eng, sems_dma)
        cnt, dcnt = P.counts
        finals = [(sems_dma[k], dcnt[k]) for k in dcnt if k.startswith("st") or k == "dbg" or (debug and k == "wq")]
        with nc.Block() as block:
            @block.sync
            def _(e):
                P.run_engine("sync", e, per_eng, final_waits=finals)

            @block.gpsimd
            def _(e):
                P.run_engine("gpsimd", e, per_eng)

            @block.vector
            def _(e):
                P.run_engine("vector", e, per_eng)

            @block.scalar
            def _(e):
                P.run_engine("scalar", e, per_eng)

            @block.tensor
            def _(e):
                P.run_engine("tensor", e, per_eng)
    return nc


def _prep_inputs(x, attn_norm_g, w_in, conv_w, sgu_w, sgu_b, sgu_norm_g, out_norm_conv_g,
                 out_norm_sgu_g, w_out, ffn_norm_g, peer_w_q, peer_sub_keys, peer_u, peer_v,
                 final_norm_g):
    f = lambda a: np.ascontiguousarray(np.asarray(a, dtype=np.float32))
    x = f(x)[0]
    col = lambda g: f(g).reshape(-1, 128).T
    cA = np.concatenate([
        col(attn_norm_g[0]), col(ffn_norm_g[0]),
        f(conv_w[0]).reshape(3, 4, 128).transpose(2, 1, 0).reshape(128, 12),
        col(sgu_norm_g[0]), col(out_norm_conv_g[0]), col(out_norm_sgu_g[0])], axis=1)
    blk = np.zeros((128, 128), np.float32)
    blk[:64, :64] = 1.0
    blk[64:, 64:] = 1.0
    cB = np.concatenate([np.eye(128, dtype=np.float32), np.ones((128, 128), np.float32), blk], axis=1)
    wT = f(np.asarray(sgu_w)[0].transpose(2, 0, 1)).reshape(128, 512)
    Bb = f(np.broadcast_to(np.asarray(sgu_b)[0][None], (128, 4, 128))).reshape(128, 512)
    gF = f(np.broadcast_to(np.asarray(final_norm_g)[None, :], (128, D)))
    keysT = f(np.asarray(peer_sub_keys)[0].transpose(3, 0, 1, 2)).reshape(128, 2048)
    UT = f(np.asarray(peer_u)[0].T)
    Vv = f(np.asarray(peer_v)[0])
    shared = {"w_in": f(w_in[0]), "w_out": f(w_out[0]), "w_q": f(peer_w_q[0]), "keysT": keysT, "UT": UT, "V": Vv,
              "cA": f(cA), "cB": cB, "wT": wT, "Bb": Bb, "gF": gF}
    in_maps = []
    for r in range(NCORES):
        xs = x[r * TOK:(r + 1) * TOK]
        xT = np.zeros((D, TOK + 2), np.float32)
        xT[:, 2:] = xs.T
        if r > 0:
            xT[:, 0:2] = x[r * TOK - 2:r * TOK].T
        m = dict(shared)
        m["x_tm"] = np.ascontiguousarray(xs)
        m["xT"] = xT
        in_maps.append(m)
    return in_maps


_NC_CACHE = {}
_DBG = {}


def kernel(**inputs):
    in_maps = _prep_inputs(**inputs)
    key = "full"
    if key not in _NC_CACHE:
        _NC_CACHE[key] = build_nc()
    nc = _NC_CACHE[key]
    res = run_bass_kernel_spmd(nc, in_maps, core_ids=list(range(NCORES)))
    out = np.concatenate([np.asarray(r["out"], dtype=np.float32) for r in res.results], axis=0)
    return out.reshape(1, SEQ, D)
```
